# Optimizing a Trainium2 kernel written in Bass

```python
import math
import jax, jax.numpy as jnp
from jax import lax
import numpy as np

D_MODEL = 1024
BATCH = 4
SEQ = 4096
DEPTH = 2

N_A_LAYERS = DEPTH // 2
N_B_LAYERS = DEPTH - N_A_LAYERS
D_FF = ((8 * D_MODEL // 3) + 127) // 128 * 128
CONV_WIDTH = 31
HEAD_DIM = 128
N_HEADS = D_MODEL // HEAD_DIM
MOBA_BLOCK = 256
MOBA_TOPK = 3
Q_CHUNK = 32
RMS_EPS = 1e-6
LN_EPS = 1e-5
MACARON_WEIGHT = 0.5
NEG_INF = -1e30

kernel_name = "yoco_conformer_conv_moba_alibi"


def rms_norm(x, g):
    x32 = x.astype(jnp.float32)
    y = x32 * lax.rsqrt(jnp.mean(x32 * x32, axis=-1, keepdims=True) + RMS_EPS)
    return (y * g.astype(jnp.float32)).astype(x.dtype)


def swiglu_ffn(h, w_gate_up, w_down):
    gate, up = jnp.split(h @ w_gate_up, 2, axis=-1)
    return (jax.nn.silu(gate) * up) @ w_down


def conformer_conv(h, w_in, b_in, w_dw, b_dw, ln_g, ln_b, w_out, b_out):
    a, g = jnp.split(h @ w_in + b_in, 2, axis=-1)
    u = a * jax.nn.sigmoid(g)
    u = lax.conv_general_dilated(
        u, w_dw[:, None, :].astype(u.dtype), window_strides=(1,),
        padding=[(CONV_WIDTH - 1, 0)],
        dimension_numbers=('NWC', 'WIO', 'NWC'),
        feature_group_count=D_MODEL) + b_dw
    u32 = u.astype(jnp.float32)
    mu = jnp.mean(u32, axis=-1, keepdims=True)
    var = jnp.mean(jnp.square(u32 - mu), axis=-1, keepdims=True)
    u = ((u32 - mu) * lax.rsqrt(var + LN_EPS) * ln_g.astype(jnp.float32)
         + ln_b.astype(jnp.float32)).astype(h.dtype)
    return jax.nn.silu(u) @ w_out + b_out


def alibi_slopes():
    return jnp.asarray(2.0 ** (-8.0 * (np.arange(N_HEADS) + 1) / N_HEADS), dtype=jnp.float32)


def shared_kv(x, g_kv, w_kv):
    B, T, _ = x.shape
    k, v = jnp.split(rms_norm(x, g_kv) @ w_kv, 2, axis=-1)
    nb = -(-T // MOBA_BLOCK)
    pad = nb * MOBA_BLOCK - T

    def to_blocks(z):
        z = jnp.pad(z, ((0, 0), (0, pad), (0, 0)))
        return z.reshape(B, nb, MOBA_BLOCK, N_HEADS, HEAD_DIM).transpose(0, 3, 1, 2, 4)

    kb = to_blocks(k)
    vb = to_blocks(v)
    k_means = jnp.mean(kb.astype(jnp.float32), axis=3).astype(kb.dtype)
    return kb, vb, k_means


def moba_attention(q, kb, vb, k_means, slopes):
    B, T = q.shape[0], q.shape[1]
    nb = kb.shape[2]
    ksel = min(MOBA_TOPK, nb)
    q = (q * (HEAD_DIM ** -0.5)).transpose(0, 2, 1, 3)
    qblk = jnp.arange(T) // MOBA_BLOCK
    gate = jnp.einsum('bhtd,bhnd->bhtn', q, k_means, preferred_element_type=jnp.float32)
    past = jnp.arange(nb)[None, :] < qblk[:, None]
    gate = jnp.where(past, gate, NEG_INF)
    _, sel = lax.top_k(gate, ksel)

    nc = T // Q_CHUNK
    q_c = q.reshape(B, N_HEADS, nc, Q_CHUNK, HEAD_DIM).transpose(2, 0, 1, 3, 4)
    sel_c = sel.reshape(B, N_HEADS, nc, Q_CHUNK, ksel).transpose(2, 0, 1, 3, 4)
    b_ix = jnp.arange(B)[:, None, None, None]
    h_ix = jnp.arange(N_HEADS)[None, :, None, None]
    slopes5 = slopes[:, None, None, None]
    slopes4 = slopes[:, None, None]

    def chunk(args):
        c, qq, ss = args
        t = c * Q_CHUNK + jnp.arange(Q_CHUNK)
        own = (c * Q_CHUNK) // MOBA_BLOCK
        valid = jnp.arange(ksel)[None, :] < (t // MOBA_BLOCK)[:, None]
        kg = kb[b_ix, h_ix, ss]
        vg = vb[b_ix, h_ix, ss]
        s_sel = jnp.einsum('bhqd,bhqkpd->bhqkp', qq, kg, preferred_element_type=jnp.float32)
        key_pos = ss[..., None] * MOBA_BLOCK + jnp.arange(MOBA_BLOCK)
        dist = (t[:, None, None] - key_pos).astype(jnp.float32)
        s_sel = jnp.where(valid[:, :, None], s_sel - slopes5 * dist, NEG_INF)
        s_sel = s_sel.reshape(B, N_HEADS, Q_CHUNK, ksel * MOBA_BLOCK)
        k_own = lax.dynamic_index_in_dim(kb, own, axis=2, keepdims=False)
        v_own = lax.dynamic_index_in_dim(vb, own, axis=2, keepdims=False)
        s_own = jnp.einsum('bhqd,bhpd->bhqp', qq, k_own, preferred_element_type=jnp.float32)
        d_own = t[:, None] - (own * MOBA_BLOCK + jnp.arange(MOBA_BLOCK))[None, :]
        s_own = jnp.where(d_own >= 0, s_own - slopes4 * d_own.astype(jnp.float32), NEG_INF)
        p = jax.nn.softmax(jnp.concatenate([s_sel, s_own], axis=-1), axis=-1)
        p_sel = p[..., :ksel * MOBA_BLOCK].reshape(B, N_HEADS, Q_CHUNK, ksel, MOBA_BLOCK).astype(vb.dtype)
        p_own = p[..., ksel * MOBA_BLOCK:].astype(vb.dtype)
        return (jnp.einsum('bhqkp,bhqkpd->bhqd', p_sel, vg)
                + jnp.einsum('bhqp,bhpd->bhqd', p_own, v_own))

    out = lax.map(chunk, (jnp.arange(nc), q_c, sel_c))
    return out.transpose(1, 0, 3, 2, 4).reshape(B, T, N_HEADS * HEAD_DIM)


def setup_inputs(seed: int = 0) -> dict:
    key = jax.random.key(seed)
    ks = jax.random.split(key, 20)
    f32 = jnp.float32

    def nrm(k, shape, scale):
        return jax.random.normal(k, shape, f32) * scale

    def gain(k, shape):
        return 1.0 + 0.02 * jax.random.normal(k, shape, f32)

    D = D_MODEL
    return {
        "x": jax.random.normal(ks[0], (BATCH, SEQ, D), f32),
        "ffn_norm_pre": gain(ks[1], (DEPTH, 2, D)),
        "ffn_norm_post": gain(ks[2], (DEPTH, 2, D)),
        "ffn_w_gate_up": nrm(ks[3], (DEPTH, 2, D, 2 * D_FF), D ** -0.5),
        "ffn_w_down": nrm(ks[4], (DEPTH, 2, D_FF, D), D_FF ** -0.5),
        "mix_norm_pre": gain(ks[5], (DEPTH, D)),
        "mix_norm_post": gain(ks[6], (DEPTH, D)),
        "conv_w_in": nrm(ks[7], (N_A_LAYERS, D, 2 * D), D ** -0.5),
        "conv_b_in": nrm(ks[8], (N_A_LAYERS, 2 * D), 0.02),
        "conv_w_dw": nrm(ks[9], (N_A_LAYERS, CONV_WIDTH, D), CONV_WIDTH ** -0.5),
        "conv_b_dw": nrm(ks[10], (N_A_LAYERS, D), 0.02),
        "conv_ln_g": gain(ks[11], (N_A_LAYERS, D)),
        "conv_ln_b": nrm(ks[12], (N_A_LAYERS, D), 0.02),
        "conv_w_out": nrm(ks[13], (N_A_LAYERS, D, D), D ** -0.5),
        "conv_b_out": nrm(ks[14], (N_A_LAYERS, D), 0.02),
        "kv_norm": gain(ks[15], (D,)),
        "w_kv": nrm(ks[16], (D, 2 * D), D ** -0.5),
        "attn_w_q": nrm(ks[17], (N_B_LAYERS, D, D), D ** -0.5),
        "attn_w_o": nrm(ks[18], (N_B_LAYERS, D, D), D ** -0.5),
    }


def reference(x, ffn_norm_pre, ffn_norm_post, ffn_w_gate_up, ffn_w_down, mix_norm_pre,
              mix_norm_post, conv_w_in, conv_b_in, conv_w_dw, conv_b_dw, conv_ln_g, conv_ln_b,
              conv_w_out, conv_b_out, kv_norm, w_kv, attn_w_q, attn_w_o):
    B, T, _ = x.shape
    slopes = alibi_slopes()
    kv = None
    for layer in range(DEPTH):
        f = swiglu_ffn(rms_norm(x, ffn_norm_pre[layer, 0]), ffn_w_gate_up[layer, 0], ffn_w_down[layer, 0])
        x = x + MACARON_WEIGHT * rms_norm(f, ffn_norm_post[layer, 0])
        h = rms_norm(x, mix_norm_pre[layer])
        if layer < N_A_LAYERS:
            a = layer
            y = conformer_conv(h, conv_w_in[a], conv_b_in[a], conv_w_dw[a], conv_b_dw[a],
                               conv_ln_g[a], conv_ln_b[a], conv_w_out[a], conv_b_out[a])
        else:
            if kv is None:
                kv = shared_kv(x, kv_norm, w_kv)
            j = layer - N_A_LAYERS
            q = (h @ attn_w_q[j]).reshape(B, T, N_HEADS, HEAD_DIM)
            y = moba_attention(q, kv[0], kv[1], kv[2], slopes) @ attn_w_o[j]
        x = x + rms_norm(y, mix_norm_post[layer])
        f = swiglu_ffn(rms_norm(x, ffn_norm_pre[layer, 1]), ffn_w_gate_up[layer, 1], ffn_w_down[layer, 1])
        x = x + MACARON_WEIGHT * rms_norm(f, ffn_norm_post[layer, 1])
    return x
```

```python
import numpy as np
import ml_dtypes
import concourse.bass as bass
import concourse.mybir as mybir
from concourse.bass_utils import run_bass_kernel_spmd

F32 = mybir.dt.float32
BF16 = mybir.dt.bfloat16
AF = mybir.ActivationFunctionType
ALU = mybir.AluOpType
AX = mybir.AxisListType

D = 1024
KC = 8
DFF = 2816
FC = 22
T = 2048
TT = 1024
SUBW = 512
HALO = 32
NBUF = HALO + TT
CONVW = 31
NH = 8
RMS_EPS = 1e-6
LN_EPS = 1e-5
NEG = -30000.0
BIG = -1e30
SEM_EPOCH = 12000


class Sched:
    ENG = ("pe", "act", "dve", "pool", "sp")

    def __init__(self):
        self.ops = {e: [] for e in self.ENG}
        self.tokw = {}
        self.tokr = {}
        self.dma_cnt = {}
        self.seen = {e: {} for e in self.ENG}
        self.pending = {e: [] for e in self.ENG}
        self.last_dma_sp = {}

    def add(self, eng, emit, reads=(), writes=(), dma_key=None):
        deps = list(self.pending[eng])
        self.pending[eng] = []
        for t in reads:
            w = self.tokw.get(t)
            if w is not None:
                deps.append(w)
            if isinstance(t, tuple) and t[0] == "ps":
                for r in self.tokr.get(t, ()):
                    if not (r[0] == "eng" and r[1] == eng):
                        deps.append(r)
        for t in writes:
            w = self.tokw.get(t)
            if w is not None:
                deps.append(w)
            deps.extend(self.tokr.get(t, ()))
        idx = len(self.ops[eng])
        if dma_key is not None:
            cnt = self.dma_cnt.get(dma_key, 0) + 1
            self.dma_cnt[dma_key] = cnt
            ref = ("dma", dma_key, cnt)
            if eng == "sp":
                self.last_dma_sp[dma_key] = ref
        else:
            ref = ("eng", eng, idx)
        need = {}
        for d in deps:
            if d[0] == "eng" and d[1] == eng and eng == "pe":
                continue
            key = (d[0], d[1])
            if d[2] > need.get(key, -1):
                need[key] = d[2]
        waits = []
        seen = self.seen[eng]
        for key, v in need.items():
            if seen.get(key, -1) >= v:
                continue
            seen[key] = v
            waits.append((key, v))
            if key[0] == "eng":
                self.ops[key[1]][v]["signal"] = True
        self.ops[eng].append(dict(emit=emit, waits=waits, signal=False, dma_key=dma_key))
        for t in reads:
            self.tokr.setdefault(t, []).append(ref)
        for t in writes:
            self.tokw[t] = ref
            self.tokr[t] = []
        return ref

    def barrier(self, engines=("pe", "act", "dve", "sp")):
        refs = []
        for e in ("pe", "act", "dve"):
            if self.ops[e]:
                refs.append(("eng", e, len(self.ops[e]) - 1))
        refs.extend(self.last_dma_sp.values())
        for e in engines:
            self.pending[e].extend(refs)

    def emit_all(self, nc, block):
        sems = {}

        def getsem(name):
            if name not in sems:
                sems[name] = nc.alloc_semaphore("s_" + name)
            return sems[name]

        sig = {}
        for e in self.ENG:
            c = 0
            arr = []
            for op in self.ops[e]:
                if op["signal"]:
                    c += 1
                arr.append(c)
            sig[e] = arr

        def waitspec(key, v):
            if key[0] == "eng":
                cnt = sig[key[1]][v]
                ep = (cnt - 1) // SEM_EPOCH
                return getsem("%s_%d" % (key[1], ep)), cnt - ep * SEM_EPOCH
            return getsem("d_" + str(key[1])), 16 * v

        handles = {"pe": block.tensor, "act": block.scalar, "dve": block.vector,
                   "pool": block.gpsimd, "sp": block.sync}
        for e in self.ENG:
            ops = self.ops[e]
            arr = sig[e]

            def body(eng, e=e, ops=ops, arr=arr):
                for i, op in enumerate(ops):
                    for key, v in op["waits"]:
                        s, val = waitspec(key, v)
                        eng.wait_ge(s, val)
                    if op["emit"] is None:
                        continue
                    ins = op["emit"](eng)
                    if op["dma_key"] is not None:
                        ins.then_inc(getsem("d_" + str(op["dma_key"])), 16)
                    elif op["signal"]:
                        ep = (arr[i] - 1) // SEM_EPOCH
                        ins.then_inc(getsem("%s_%d" % (e, ep)), 1)

            handles[e](body)


class Cols:
    def __init__(self):
        self.n = 0
        self.m = {}

    def add(self, name, w):
        self.m[name] = self.n
        self.n += w
        return self.m[name]


CC = Cols()
for _l in range(2):
    for _i in range(2):
        CC.add(("fpre", _l, _i), KC)
        CC.add(("fpost", _l, _i), KC)
    CC.add(("mpre", _l), KC)
    CC.add(("mpost", _l), KC)
for _n in ("bin_a", "bin_g", "bdw", "lng", "lnb", "bout", "kvn"):
    CC.add(_n, KC)
CC.add("wdw", KC * CONVW)
N_IN_COLS = CC.n
for _l in range(2):
    for _i in range(2):
        CC.add(("fposth", _l, _i), KC)
CC.add("boutg", KC)
CC.add("flag", 1)
N_COLS = CC.n

ALIBI_SLOPES = [2.0 ** (-8.0 * (h + 1) / NH) for h in range(NH)]


def host_consts(inp):
    def pc(v):
        return np.ascontiguousarray(np.asarray(v, np.float32).reshape(-1, 128).T)

    c = np.zeros((128, N_IN_COLS), np.float32)

    def put(name, v):
        a = pc(v)
        c[:, CC.m[name]:CC.m[name] + a.shape[1]] = a

    for l in range(2):
        for i in range(2):
            put(("fpre", l, i), inp["ffn_norm_pre"][l, i])
            put(("fpost", l, i), inp["ffn_norm_post"][l, i])
        put(("mpre", l), inp["mix_norm_pre"][l])
        put(("mpost", l), inp["mix_norm_post"][l])
    put("bin_a", inp["conv_b_in"][0, :D])
    put("bin_g", inp["conv_b_in"][0, D:])
    put("bdw", inp["conv_b_dw"][0])
    put("lng", inp["conv_ln_g"][0])
    put("lnb", inp["conv_ln_b"][0])
    put("bout", inp["conv_b_out"][0])
    put("kvn", inp["kv_norm"])
    wdw = np.asarray(inp["conv_w_dw"][0], np.float32)
    a = wdw.T.reshape(KC, 128, CONVW).transpose(1, 0, 2).reshape(128, KC * CONVW)
    c[:, CC.m["wdw"]:CC.m["wdw"] + KC * CONVW] = a
    return c


def attn_consts():
    bf = ml_dtypes.bfloat16
    ident = np.eye(128, dtype=np.float32).astype(bf)
    ones = np.ones((128, 128), np.float32).astype(bf)
    E = np.zeros((128, 17, 128), np.float32)
    for j in range(16):
        E[j, j, :] = 1.0
        E[16, j, :] = 1.0
    E[16, 16, :] = 1.0
    E = E.astype(bf)
    causal = np.zeros((128, 2, 256), np.float32)
    p = np.arange(128)[:, None]
    q = np.arange(256)[None, :]
    for kt in range(2):
        causal[:, kt, :] = np.where(q >= kt * 128 + p, 0.0, NEG)
    causal = causal.astype(bf)
    abias = np.zeros((128, NH, 34), np.float32)
    for h in range(NH):
        for o in range(34):
            abias[:, h, o] = ALIBI_SLOPES[h] * (np.arange(128) + 128.0 * (o - 32))
    rowv = np.zeros((128, NH, 2), np.float32)
    for h in range(NH):
        for half in range(2):
            rowv[:, h, half] = -ALIBI_SLOPES[h] * (half * 128.0 + np.arange(128))
    pst = np.zeros((128, 8, 16), np.float32)
    for qt in range(8):
        pst[:, qt, 8 + qt:] = BIG
    return dict(c_ident=ident, c_ones=ones, c_E=E, c_causal=causal, c_abias=abias,
                c_rowv=rowv, c_pst=pst)


class Sub:
    def __init__(self, name, col, w, xcol):
        self.name = name
        self.col = col
        self.w = w
        self.xcol = xcol


class Prog:
    def __init__(self, mode):
        self.mode = mode
        nc = bass.Bass("TRN2", target_bir_lowering=False)
        self.nc = nc
        self.S = Sched()
        S = self.S
        A = mode in ("A", "AB")
        B = mode in ("B", "AB")

        def din(name, shape, dt=F32):
            return nc.dram_tensor(name, list(shape), dt, kind="ExternalInput").ap()

        def dout(name, shape, dt=F32):
            return nc.dram_tensor(name, list(shape), dt, kind="ExternalOutput").ap()

        self.d_x = din("xin", [128, KC, T])
        self.d_consts = din("consts", [128, N_IN_COLS])
        self.d_flag = din("flag", [128, 1])
        self.d_wgu = din("ffn_w_gate_up", [2, 2, D, 2 * DFF])
        self.d_wd = din("ffn_w_down", [2, 2, DFF, D])
        self.d_ones = din("c_ones", [128, 128], BF16)
        if A:
            self.d_xh = din("xh", [128, KC, HALO])
            self.d_cwin = din("conv_w_in", [1, D, 2 * D])
            self.d_cwout = din("conv_w_out", [1, D, D])
            self.d_wkv = din("w_kv", [D, 2 * D])
            self.d_ident = din("c_ident", [128, 128], BF16)
        if B:
            self.d_wq = din("attn_w_q", [1, D, D])
            self.d_wo = din("attn_w_o", [1, D, D])
            if not A:
                self.d_ident = din("c_ident", [128, 128], BF16)
            self.d_E = din("c_E", [128, 17, 128], BF16)
            self.d_causal = din("c_causal", [128, 2, 256], BF16)
            self.d_abias = din("c_abias", [128, NH, 34])
            self.d_rowv = din("c_rowv", [128, NH, 2])
            self.d_pst = din("c_pst", [128, 8, 16])
            self.d_gmask = din("gmask", [128, 16])
        if mode == "A":
            self.d_xout = dout("xa", [128, KC, T])
            self.d_kout = dout("kout", [NH, 128, T], BF16)
            self.d_vout = dout("vout", [T, D], BF16)
        if mode == "B":
            self.d_kin = din("kin", [NH, 128, 2 * T], BF16)
            self.d_vin = din("vin", [2 * T, D], BF16)
            self.d_xout = dout("xout", [128, KC, T])

        self.X = nc.alloc_sbuf_tensor("sb_X", [128, KC, T], F32)
        self.cst = nc.alloc_sbuf_tensor("sb_cst", [128, N_COLS], F32)
        self.ones = nc.alloc_sbuf_tensor("sb_ones", [128, 128], BF16)
        self.ident = nc.alloc_sbuf_tensor("sb_ident", [128, 128], BF16)
        self.wgu = [nc.alloc_sbuf_tensor("sb_wgu%d" % i, [128, KC, 2, 128], BF16) for i in range(3)]
        self.wd = [nc.alloc_sbuf_tensor("sb_wd%d" % i, [128, FC, 128], BF16) for i in range(2)]
        if A:
            self.xh = nc.alloc_sbuf_tensor("sb_xh", [128, KC, HALO], F32)
            self.uhs = nc.alloc_sbuf_tensor("sb_uhs", [128, KC, HALO], BF16)
            self.wvbuf = nc.alloc_sbuf_tensor("sb_wvbuf", [128, KC, SUBW], BF16)
        if B:
            self.cE = nc.alloc_sbuf_tensor("sb_cE", [128, 17, 128], BF16)
            self.ccausal = nc.alloc_sbuf_tensor("sb_ccausal", [128, 2, 256], BF16)
            self.cabias = nc.alloc_sbuf_tensor("sb_cabias", [128, NH, 34], F32)
            self.crowv = nc.alloc_sbuf_tensor("sb_crowv", [128, NH, 2], F32)
            self.cpmask = nc.alloc_sbuf_tensor("sb_cpmask", [128, 8, 16], F32)
            self.cgmask = nc.alloc_sbuf_tensor("sb_cgmask", [128, 16], F32)
        self.ARENA_BYTES = 106 * 1024
        self.arena = nc.alloc_sbuf_tensor("sb_arena", [128, self.ARENA_BYTES // 4], F32)
        self.ps = nc.alloc_psum_tensor("pp_ps", [128, 8, 512], F32)
        self.aoff = 0
        self.wgu_ctr = 0
        self.wd_ctr = 0
        self.bank4 = 0
        self.uid = 0

        with nc.Block() as block:
            self.build(A, B)
            S.emit_all(nc, block)

    def carve_reset(self, keep=0):
        self.aoff = keep

    def carve(self, dt, shape):
        esz = 2 if dt == BF16 else 4
        n = int(np.prod(shape))
        nbytes = (n * esz + 63) // 64 * 64
        assert self.aoff + nbytes <= self.ARENA_BYTES, ("arena overflow", self.aoff, nbytes)
        a = self.arena[:, self.aoff // 4:(self.aoff + nbytes) // 4]
        self.aoff += nbytes
        if dt == BF16:
            a = a.bitcast(BF16)
        a = a[:, 0:n]
        if len(shape) == 2:
            a = a.rearrange("p (a b) -> p a b", a=shape[0])
        elif len(shape) == 3:
            a = a.rearrange("p (a b c) -> p a b c", a=shape[0], b=shape[1])
        return a

    def bank(self, b):
        return self.ps[:, b, :]

    def nextbank4(self):
        b = self.bank4
        self.bank4 = (b + 1) % 4
        return b

    def ccol(self, name, k=0):
        c = CC.m[name] + k
        return self.cst[:, c:c + 1]

    def xv(self, sub, k):
        if sub.xcol is None:
            return self.xh[:, k, :]
        return self.X[:, k, sub.xcol:sub.xcol + sub.w]

    def xtok(self, sub, k):
        if sub.xcol is None:
            return ("xh", k)
        return ("x", k, sub.xcol // SUBW)

    def subs(self, tile, halo):
        r = []
        if halo:
            r.append(Sub("h", 0, HALO, None))
        for s in range(2):
            r.append(Sub(s, HALO + s * SUBW, SUBW, tile * TT + s * SUBW))
        return r

    def mm(self, out, lhsT, rhs, start, stop, reads, writes):
        self.S.add("pe", lambda e: e.matmul(out, lhsT=lhsT, rhs=rhs, start=start, stop=stop),
                   reads, writes)

    def act(self, out, in_, func, reads, writes, bias=None, scale=None):
        kw = {}
        if bias is not None:
            kw["bias"] = bias
        if scale is not None:
            kw["scale"] = scale
        self.S.add("act", lambda e: e.activation(out=out, in_=in_, func=func, **kw), reads, writes)

    def dve(self, fn, reads, writes):
        self.S.add("dve", fn, reads, writes)

    def dma(self, q, out, in_, key, reads, writes):
        self.S.add(q, lambda e: e.dma_start(out=out, in_=in_), reads, writes, dma_key=key)

    def norm_stats(self, sqv, w, sqtoks, eps, scale=1.0 / D):
        for k in range(KC):
            self.mm(self.bank(4)[:, :w], self.ones[:, :], sqv(k), k == 0, k == KC - 1,
                    [sqtoks(k), "ones"], [("ps", 4)])
        rs = self.rs[:, :w]
        self.act(rs, self.bank(4)[:, :w], AF.Sqrt, [("ps", 4)], ["rs"], bias=self.eps_ap(eps), scale=scale)
        r5 = self.bank(5)[:, :w]
        self.dve(lambda e: e.reciprocal(out=r5, in_=rs), ["rs"], [("ps", 5)])

    def eps_ap(self, eps):
        return self.epsc[:, 0:1] if eps == RMS_EPS else self.epsc[:, 1:2]

    def prenorm(self, sub, gains, outs, outtoks):
        w = sub.w
        sq = self.sq[sub.name]
        sname = ("sq", sub.name)
        for k in range(KC):
            self.act(sq[:, k, :w], self.xv(sub, k), AF.Square, [self.xtok(sub, k)], [(sname, k)])
        self.norm_stats(lambda k: sq[:, k, :w], w, lambda k: (sname, k), RMS_EPS)
        r5 = self.bank(5)[:, :w]
        for g, out, otok in zip(gains, outs, outtoks):
            for k in range(KC):
                o = out[:, k, sub.col:sub.col + w]
                xin = self.xv(sub, k)
                gc = self.ccol(g, k)
                self.dve(lambda e, o=o, xin=xin, gc=gc: e.scalar_tensor_tensor(
                    out=o, in0=xin, scalar=gc, in1=r5, op0=ALU.mult, op1=ALU.mult),
                    [self.xtok(sub, k), ("ps", 5), "cst"], [(otok, k, sub.name)])

    def load_pair(self, wsrc, colA, colB):
        slot = self.wgu_ctr % 3
        self.wgu_ctr += 1
        for g, c0 in enumerate((colA, colB)):
            src = wsrc[:, c0:c0 + 128].rearrange("(kc p) n -> p kc n", p=128)
            self.dma("pool", self.wgu[slot][:, :, g, :], src, "wgu%d_%d" % (slot, g), [], [("wgu", slot, g)])
        return slot

    def proj_in(self, hbuf, htok, subs, wsrc, pairs, evac):
        n = len(pairs)
        slots = {}
        for p in range(min(2, n)):
            slots[p] = self.load_pair(wsrc, *pairs[p])
        for p in range(n):
            if p + 2 < n:
                slots[p + 2] = self.load_pair(wsrc, *pairs[p + 2])
            slot = slots[p]
            for sub in subs:
                banks = []
                for g in range(2):
                    b = self.nextbank4()
                    banks.append(b)
                    for k in range(KC):
                        self.mm(self.bank(b)[:, :sub.w], self.wgu[slot][:, k, g, :],
                                hbuf[:, k, sub.col:sub.col + sub.w], k == 0, k == KC - 1,
                                [("wgu", slot, g), (htok, k, sub.name)], [("ps", b)])
                evac(p, sub, banks[0], banks[1])

    def load_wd(self, wsrc, C, m):
        slot = self.wd_ctr % 2
        self.wd_ctr += 1
        src = wsrc[:, m * 128:(m + 1) * 128].rearrange("(c p) n -> p c n", p=128)
        self.dma("pool", self.wd[slot][:, 0:C, :], src, "wd%d" % slot, [], [("wd", slot)])
        return slot

    def proj_out(self, mov, movtoks, C, subs, wsrc, gname, bgname=None, bname=None):
        slots = {0: self.load_wd(wsrc, C, 0)}
        for m in range(KC):
            if m + 1 < KC:
                slots[m + 1] = self.load_wd(wsrc, C, m + 1)
            slot = slots[m]
            for sub in subs:
                w = sub.w
                b = self.nextbank4()
                for c in range(C):
                    self.mm(self.bank(b)[:, :w], self.wd[slot][:, c, :], mov[:, c, sub.col:sub.col + w],
                            c == 0, c == C - 1, [("wd", slot)] + movtoks(c, sub), [("ps", b)])
                ysb = self.ysb[sub.name]
                ysq = self.sq[sub.name]
                self.act(ysb[:, m, :w], self.bank(b)[:, :w], AF.Identity, [("ps", b), "cst"],
                         [("ysb", sub.name, m)], scale=self.ccol(gname, m),
                         bias=(self.ccol(bgname, m) if bgname else None))
                self.act(ysq[:, m, :w], self.bank(b)[:, :w], AF.Square, [("ps", b), "cst"],
                         [(("sq", sub.name), m)], bias=(self.ccol(bname, m) if bname else None))
        for sub in subs:
            w = sub.w
            ysb = self.ysb[sub.name]
            ysq = self.sq[sub.name]
            sname = ("sq", sub.name)
            self.norm_stats(lambda k: ysq[:, k, :w], w, lambda k: (sname, k), RMS_EPS)
            r5 = self.bank(5)[:, :w]
            for m in range(KC):
                j = self.uid % 2
                self.uid += 1
                tmp = self.tmp[j][:, :w]
                ym = ysb[:, m, :w]
                self.dve(lambda e, tmp=tmp, ym=ym, r5=r5: e.tensor_tensor(out=tmp, in0=ym, in1=r5, op=ALU.mult),
                         [("ysb", sub.name, m), ("ps", 5)], [("tmp", j)])
                xo = self.xv(sub, m)
                self.dve(lambda e, xo=xo, tmp=tmp: e.tensor_tensor(out=xo, in0=xo, in1=tmp, op=ALU.add),
                         [("tmp", j), self.xtok(sub, m)], [self.xtok(sub, m)])

    def carve_common(self, subs):
        self.rs = self.carve(F32, [SUBW])
        self.tmp = [self.carve(F32, [SUBW]) for _ in range(2)]
        self.sg = [self.carve(BF16, [SUBW]) for _ in range(2)]

    def ffn(self, l, i, tile, halo):
        S = self.S
        subs = self.subs(tile, halo)
        S.barrier()
        self.carve_reset(self.akeep)
        hT = self.carve(BF16, [KC, NBUF])
        off_h = self.aoff
        actb = self.carve(BF16, [FC, NBUF])
        self.sq = {0: self.carve(BF16, [KC, SUBW]), 1: self.carve(BF16, [KC, SUBW])}
        self.ysb = {1: self.carve(F32, [KC, SUBW])}
        if halo:
            self.sq["h"] = self.carve(BF16, [KC, HALO])
            self.ysb["h"] = self.carve(F32, [KC, HALO])
        self.carve_common(subs)
        for sub in subs:
            self.prenorm(sub, [("fpre", l, i)], [hT], ["hT"])

        def evac(c, sub, bA, bB):
            w = sub.w
            j = self.uid % 2
            self.uid += 1
            sg = self.sg[j][:, :w]
            self.act(sg, self.bank(bA)[:, :w], AF.Silu, [("ps", bA)], [("sg", j)])
            o = actb[:, c, sub.col:sub.col + w]
            pb = self.bank(bB)[:, :w]
            self.dve(lambda e: e.tensor_tensor(out=o, in0=sg, in1=pb, op=ALU.mult),
                     [("sg", j), ("ps", bB)], [("act", c, sub.name)])

        pairs = [(c * 128, DFF + c * 128) for c in range(FC)]
        self.proj_in(hT, "hT", subs, self.d_wgu[l, i], pairs, evac)
        S.barrier()
        save = self.aoff
        self.aoff = off_h - ((KC * NBUF * 2 + 63) // 64 * 64)
        self.ysb[0] = self.carve(F32, [KC, SUBW])
        self.aoff = save
        self.proj_out(actb, lambda c, sub: [("act", c, sub.name)], FC, subs, self.d_wd[l, i],
                      ("fposth", l, i))

    def conv(self, tile):
        S = self.S
        halo = True
        subs = self.subs(tile, halo)
        main = subs[1:]
        S.barrier()
        self.carve_reset(self.akeep)
        hT = self.carve(BF16, [KC, NBUF])
        off_after_h = self.aoff
        u = self.carve(BF16, [KC, NBUF])
        sbf = {0: self.carve(BF16, [KC, SUBW]), 1: self.carve(BF16, [KC, SUBW])}
        v = self.carve(F32, [KC, SUBW])
        self.sq = {0: self.carve(BF16, [KC, SUBW]), 1: self.carve(BF16, [KC, SUBW]),
                   "h": self.carve(BF16, [KC, HALO])}
        mu_dummy = None
        self.carve_common(subs)
        tf = [self.carve(F32, [SUBW]) for _ in range(2)]
        psubs = subs if tile == 0 else main
        for sub in psubs:
            self.prenorm(sub, [("mpre", 0)], [hT], ["hT"])
        if tile == 1:
            uh = u[:, :, 0:HALO]
            uhs = self.uhs[:, :, :]
            self.dve(lambda e: e.tensor_copy(out=uh, in_=uhs), ["uhs"], [("u", k, "h") for k in range(KC)])

        def evac(j, sub, bA, bB):
            w = sub.w
            jj = self.uid % 2
            self.uid += 1
            sgf = tf[jj][:, :w]
            self.act(sgf, self.bank(bB)[:, :w], AF.Sigmoid, [("ps", bB), "cst"], [("tf", jj)],
                     bias=self.ccol("bin_g", j))
            o = u[:, j, sub.col:sub.col + w]
            pa = self.bank(bA)[:, :w]
            ba = self.ccol("bin_a", j)
            self.dve(lambda e: e.scalar_tensor_tensor(out=o, in0=pa, scalar=ba, in1=sgf,
                                                      op0=ALU.add, op1=ALU.mult),
                     [("tf", jj), ("ps", bA), "cst"], [("u", j, sub.name)])
            if sub.name == "h":
                fl = self.ccol("flag")
                self.dve(lambda e: e.tensor_scalar(out=o, in0=o, scalar1=fl, scalar2=None, op0=ALU.mult),
                         [("u", j, "h"), "cst"], [("u", j, "h")])

        pairs = [(j * 128, D + j * 128) for j in range(KC)]
        self.proj_in(hT, "hT", psubs, self.d_cwin[0], pairs, evac)
        S.barrier()
        save = self.aoff
        self.aoff = off_after_h - ((KC * NBUF * 2 + 63) // 64 * 64)
        dg = [self.carve(BF16, [CONVW, 128]) for _ in range(2)]
        self.aoff = save
        for sub in main:
            for j in range(KC):
                d = dg[j % 2]
                wv = self.cst[:, CC.m["wdw"] + j * CONVW: CC.m["wdw"] + (j + 1) * CONVW]
                idb = self.ident[:, :].unsqueeze(1).to_broadcast([128, CONVW, 128])
                wb = wv.unsqueeze(2).to_broadcast([128, CONVW, 128])
                self.dve(lambda e, d=d, idb=idb, wb=wb: e.tensor_tensor(out=d, in0=idb, in1=wb, op=ALU.mult),
                         ["cst", "ident"], [("dg", j % 2)])
                b = self.nextbank4()
                for k in range(CONVW):
                    c0 = sub.col - (CONVW - 1) + k
                    rd = [("dg", j % 2), ("u", j, sub.name)]
                    if sub.name == 0:
                        rd.append(("u", j, "h"))
                    else:
                        rd.append(("u", j, 0))
                    self.mm(self.bank(b)[:, :SUBW], d[:, k, :], u[:, j, c0:c0 + SUBW], k == 0, k == CONVW - 1,
                            rd, [("ps", b)])
                bd = self.ccol("bdw", j)
                self.act(v[:, j, :], self.bank(b)[:, :], AF.Identity, [("ps", b), "cst"], [("v", j)], bias=bd)
                self.act(self.sq[0][:, j, :], self.bank(b)[:, :], AF.Square, [("ps", b), "cst"],
                         [(("sq", 0), j)], bias=bd)
                vb = self.sq[1][:, j, :]
                pb = self.bank(b)[:, :]
                self.dve(lambda e, vb=vb, pb=pb, bd=bd: e.tensor_scalar(out=vb, in0=pb, scalar1=bd, scalar2=None,
                                                                        op0=ALU.add),
                         [("ps", b), "cst"], [(("sq", 1), j)])
            for k in range(KC):
                self.mm(self.bank(4)[:, :], self.ones[:, :], self.sq[1][:, k, :], k == 0, k == KC - 1,
                        [(("sq", 1), k), "ones"], [("ps", 4)])
            for k in range(KC):
                self.mm(self.bank(6)[:, :], self.ones[:, :], self.sq[0][:, k, :], k == 0, k == KC - 1,
                        [(("sq", 0), k), "ones"], [("ps", 6)])
            b4, b5, b6, b7 = self.bank(4)[:, :], self.bank(5)[:, :], self.bank(6)[:, :], self.bank(7)[:, :]
            self.dve(lambda e: e.tensor_scalar(out=b7, in0=b4, scalar1=1.0 / D, scalar2=None, op0=ALU.mult),
                     [("ps", 4)], [("ps", 7)])
            t0 = tf[0][:, :]
            t1 = tf[1][:, :]
            self.act(t0, b4, AF.Square, [("ps", 4)], [("tf", 0)], scale=1.0 / D)
            self.dve(lambda e: e.scalar_tensor_tensor(out=t1, in0=b6, scalar=1.0 / D, in1=t0,
                                                      op0=ALU.mult, op1=ALU.subtract),
                     [("ps", 6), ("tf", 0)], [("tf", 1)])
            rs = self.rs[:, :]
            self.act(rs, t1, AF.Sqrt, [("tf", 1)], ["rs"], bias=self.eps_ap(LN_EPS), scale=1.0)
            self.dve(lambda e: e.reciprocal(out=b5, in_=rs), ["rs"], [("ps", 5)])
            for j in range(KC):
                jj = self.uid % 2
                self.uid += 1
                ta = self.tmp[jj][:, :]
                vj = v[:, j, :]
                self.dve(lambda e, ta=ta, vj=vj: e.tensor_tensor(out=ta, in0=vj, in1=b7, op=ALU.subtract),
                         [("v", j), ("ps", 7)], [("tmp", jj)])
                self.dve(lambda e, ta=ta: e.tensor_tensor(out=ta, in0=ta, in1=b5, op=ALU.mult),
                         [("tmp", jj), ("ps", 5)], [("tmp", jj)])
                self.act(sbf[sub.name][:, j, :], ta, AF.Silu, [("tmp", jj), "cst"], [("sbf", j, sub.name)],
                         scale=self.ccol("lng", j), bias=self.ccol("lnb", j))
        if tile == 0:
            ul = u[:, :, TT:TT + HALO]
            uhs = self.uhs[:, :, :]
            self.dve(lambda e: e.tensor_copy(out=uhs, in_=ul), [("u", k, 1) for k in range(KC)], ["uhs"])
        S.barrier()
        save = self.aoff
        self.aoff = off_after_h - ((KC * NBUF * 2 + 63) // 64 * 64)
        self.ysb = {0: self.carve(F32, [KC, SUBW]), 1: v}
        self.aoff = save
        mov = None

        class MovView:
            def __getitem__(s2, idx):
                raise NotImplementedError

        self._proj_out_sbf(sbf, main)

    def _proj_out_sbf(self, sbf, main):
        C = KC
        wsrc = self.d_cwout[0]
        slots = {0: self.load_wd(wsrc, C, 0)}
        for m in range(KC):
            if m + 1 < KC:
                slots[m + 1] = self.load_wd(wsrc, C, m + 1)
            slot = slots[m]
            for sub in main:
                b = self.nextbank4()
                for c in range(C):
                    self.mm(self.bank(b)[:, :], self.wd[slot][:, c, :], sbf[sub.name][:, c, :],
                            c == 0, c == C - 1, [("wd", slot), ("sbf", c, sub.name)], [("ps", b)])
                ysb = self.ysb[sub.name]
                ysq = self.sq[sub.name]
                self.act(ysb[:, m, :], self.bank(b)[:, :], AF.Identity, [("ps", b), "cst"],
                         [("ysb", sub.name, m)], scale=self.ccol(("mpost", 0), m), bias=self.ccol("boutg", m))
                self.act(ysq[:, m, :], self.bank(b)[:, :], AF.Square, [("ps", b), "cst"],
                         [(("sq", sub.name), m)], bias=self.ccol("bout", m))
        self._post(main)

    def _post(self, subs):
        for sub in subs:
            w = sub.w
            ysb = self.ysb[sub.name]
            ysq = self.sq[sub.name]
            sname = ("sq", sub.name)
            self.norm_stats(lambda k: ysq[:, k, :w], w, lambda k: (sname, k), RMS_EPS)
            r5 = self.bank(5)[:, :w]
            for m in range(KC):
                j = self.uid % 2
                self.uid += 1
                tmp = self.tmp[j][:, :w]
                ym = ysb[:, m, :w]
                self.dve(lambda e, tmp=tmp, ym=ym, r5=r5: e.tensor_tensor(out=tmp, in0=ym, in1=r5, op=ALU.mult),
                         [("ysb", sub.name, m), ("ps", 5)], [("tmp", j)])
                xo = self.xv(sub, m)
                self.dve(lambda e, xo=xo, tmp=tmp: e.tensor_tensor(out=xo, in0=xo, in1=tmp, op=ALU.add),
                         [("tmp", j), self.xtok(sub, m)], [self.xtok(sub, m)])

    def kvq(self, tile, do_kv, do_q):
        S = self.S
        subs = self.subs(tile, False)
        S.barrier()
        self.carve_reset(self.akeep)
        hT = self.carve(BF16, [KC, NBUF])
        hkv = self.carve(BF16, [KC, NBUF])
        self.sq = {0: self.carve(BF16, [KC, SUBW]), 1: self.carve(BF16, [KC, SUBW])}
        self.carve_common(subs)
        kst = [self.carve(BF16, [SUBW]) for _ in range(2)]
        for sub in subs:
            self.prenorm(sub, [("mpre", 1), "kvn"], [hT, hkv], ["hT", "hkv"])
        if do_kv:
            def evac_k(p, sub, bA, bB):
                for b, h in ((bA, 2 * p), (bB, 2 * p + 1)):
                    j = self.uid % 2
                    self.uid += 1
                    self.act(kst[j][:, :], self.bank(b)[:, :], AF.Copy, [("ps", b)], [("kst", j)])
                    self.dma("sp", self.d_kout[h, :, sub.xcol:sub.xcol + SUBW], kst[j][:, :], "kst%d" % j,
                             [("kst", j)], [])
            self.proj_in(hkv, "hkv", subs, self.d_wkv, [(2 * j * 128, (2 * j + 1) * 128) for j in range(4)],
                         evac_k)
            wv = self.wvbuf
            vst = [self.carve(BF16, [SUBW]) for _ in range(2)]
            for half in range(2):
                src = self.d_wkv[:, D + half * SUBW: D + (half + 1) * SUBW].rearrange("(kc p) n -> p kc n", p=128)
                self.dma("pool", wv[:, :, :], src, "wv", [], ["wv"])
                for sub in subs:
                    for tb in range(SUBW // 128):
                        b = self.nextbank4()
                        c0 = sub.col + tb * 128
                        for k in range(KC):
                            self.mm(self.bank(b)[:, :], hkv[:, k, c0:c0 + 128], wv[:, k, :], k == 0, k == KC - 1,
                                    [("hkv", k, sub.name), "wv"], [("ps", b)])
                        j = self.uid % 2
                        self.uid += 1
                        self.act(vst[j][:, :], self.bank(b)[:, :], AF.Copy, [("ps", b)], [("vst", j)])
                        r0 = sub.xcol + tb * 128
                        self.dma("sp", self.d_vout[r0:r0 + 128, half * SUBW:(half + 1) * SUBW], vst[j][:, :],
                                 "vst%d" % j, [("vst", j)], [])
        if do_q:
            def evac_q(p, sub, bA, bB):
                for b, h in ((bA, 2 * p), (bB, 2 * p + 1)):
                    o = self.QA[:, h, sub.xcol:sub.xcol + SUBW]
                    qts = [("qa", h, sub.xcol // 256), ("qa", h, sub.xcol // 256 + 1)]
                    self.act(o, self.bank(b)[:, :], AF.Copy, [("ps", b)], qts, scale=float(128 ** -0.5))
            self.proj_in(hT, "hT", subs, self.d_wq[0], [(2 * j * 128, (2 * j + 1) * 128) for j in range(4)],
                         evac_q)

    def attention(self):
        S = self.S
        S.barrier()
        self.carve_reset(self.akeep)
        Kb = [self.carve(BF16, [2 * T]) for _ in range(2)]
        Vb = [self.carve(BF16, [32, 128]) for _ in range(2)]
        PT = [self.carve(BF16, [256]) for _ in range(4)]
        kmf = [self.carve(F32, [16]) for _ in range(2)]
        kmb = [self.carve(BF16, [16]) for _ in range(2)]
        gm = [self.carve(F32, [16]) for _ in range(2)]
        top8 = [self.carve(F32, [8]) for _ in range(2)]
        thr = [self.carve(F32, [1]) for _ in range(2)]
        seln = [self.carve(BF16, [128]) for _ in range(2)]
        mneg = [self.carve(BF16, [256]) for _ in range(2)]
        rden = [self.carve(F32, [256]) for _ in range(2)]
        for j in range(2):
            s_ = seln[j][:, :]
            self.dve(lambda e, s_=s_: e.memset(s_, 0.0), [], [("seln", j)])

        def load_head(h):
            hb = h % 2
            self.dma("sp", Kb[hb][:, :], self.d_kin[h, :, :], "kb%d" % hb, [], [("kb", hb)])
            src = self.d_vin[:, h * 128:(h + 1) * 128].rearrange("(kk p) d -> p kk d", p=128)
            self.dma("sp", Vb[hb][:, :, :], src, "vb%d" % hb, [], [("vb", hb)])

        load_head(0)
        it = 0
        for h in range(NH):
            hb = h % 2
            if h + 1 < NH:
                load_head(h + 1)
            kv3 = Kb[hb][:, :].rearrange("p (j s) -> p j s", s=256)
            kf = kmf[hb][:, :]
            self.dve(lambda e, kf=kf, kv3=kv3: e.tensor_reduce(out=kf, in_=kv3, axis=AX.X, op=ALU.add),
                     [("kb", hb)], [("kmf", hb)])
            kbm = kmb[hb][:, :]
            self.dve(lambda e, kbm=kbm, kf=kf: e.tensor_scalar(out=kbm, in0=kf, scalar1=1.0 / 256, scalar2=None,
                                                              op0=ALU.mult),
                     [("kmf", hb)], [("kmb", hb)])
            for qt in range(8):
                own = 8 + qt
                q0 = qt * 256
                qv = self.QA[:, h, q0:q0 + 256]
                qtok = ("qa", h, qt)
                ib = it % 2
                it += 1
                bt = self.nextbank4()
                btb = self.bank(bt).bitcast(BF16)
                for half in range(2):
                    bg = self.nextbank4()
                    self.mm(self.bank(bg)[:, 0:16], self.QA[:, h, q0 + half * 128:q0 + (half + 1) * 128], kbm,
                            True, True, [qtok, ("kmb", hb)], [("ps", bg)])
                    g_ = gm[half][:, :]
                    pg = self.bank(bg)[:, 0:16]
                    pm = self.cpmask[:, qt, :]
                    self.dve(lambda e, g_=g_, pg=pg, pm=pm: e.tensor_tensor(out=g_, in0=pg, in1=pm, op=ALU.add),
                             [("ps", bg), "cpmask"], [("gm", half)])
                    t8 = top8[half][:, :]
                    self.dve(lambda e, t8=t8, g_=g_: e.max(out=t8, in_=g_), [("gm", half)], [("top8", half)])
                    th = thr[half][:, :]
                    t83 = top8[half][:, 2:3]
                    self.dve(lambda e, th=th, t83=t83: e.tensor_scalar(out=th, in0=t83, scalar1=-1e29, scalar2=None,
                                                                      op0=ALU.max),
                             [("top8", half)], [("thr", half)])
                    sl = seln[half][:, 0:16]
                    self.dve(lambda e, sl=sl, g_=g_, th=th: e.tensor_scalar(out=sl, in0=g_, scalar1=th, scalar2=NEG,
                                                                           op0=ALU.is_lt, op1=ALU.mult),
                             [("gm", half), ("thr", half)], [("seln", half)])
                    s16 = seln[half][:, 16:17]
                    rv = self.crowv[:, h, half:half + 1]
                    self.dve(lambda e, s16=s16, rv=rv: e.tensor_copy(out=s16, in_=rv), ["crowv", ("seln", half)],
                             [("seln", half)])
                    sfull = seln[half][:, :]
                    self.S.add("pe", lambda e, o=btb[:, half * 128:(half + 1) * 128], sfull=sfull:
                               e.transpose(o, sfull, self.ident[:, :]),
                               [("seln", half), "ident"], [("ps", bt)])
                mn = mneg[ib][:, :]
                self.act(mn, btb[:, 0:256], AF.Copy, [("ps", bt)], [("mneg", ib)])
                bO = 4 + (it % 2)
                bD = 6 + (it % 2)
                ntile = 2 * (own + 1)
                for kk in range(ntile):
                    j = kk // 2
                    kt = kk % 2
                    b = self.nextbank4()
                    diag = (j == own)
                    self.mm(self.bank(b)[:, :256], Kb[hb][:, kk * 128:(kk + 1) * 128], qv, True, False,
                            [("kb", hb), qtok], [("ps", b)])
                    if not diag:
                        self.mm(self.bank(b)[:, :256], self.cE[:, j, :], mn, False, True,
                                ["cE", ("mneg", ib)], [("ps", b)])
                    else:
                        self.mm(self.bank(b)[:, :256], self.cE[:, 16, :], mn, False, False,
                                ["cE", ("mneg", ib)], [("ps", b)])
                        self.mm(self.bank(b)[:, :256], self.ident[:, :], self.ccausal[:, kt, :], False, True,
                                ["ident", "ccausal"], [("ps", b)])
                    pj = self.uid % 4
                    self.uid += 1
                    pt = PT[pj][:, :]
                    off = kk - 2 * own + 32
                    self.act(pt, self.bank(b)[:, :256], AF.Exp, [("ps", b), "cabias"], [("pt", pj)],
                             bias=self.cabias[:, h, off:off + 1])
                    self.mm(self.bank(bO)[:, :256], Vb[hb][:, kk, :], pt, kk == 0, kk == ntile - 1,
                            [("vb", hb), ("pt", pj)], [("ps", bO)])
                    self.mm(self.bank(bD)[:, :256], self.ones[:, :], pt, kk == 0, kk == ntile - 1,
                            ["ones", ("pt", pj)], [("ps", bD)])
                rd = rden[ib][:, :]
                pD = self.bank(bD)[:, :256]
                pO = self.bank(bO)[:, :256]
                self.dve(lambda e, rd=rd, pD=pD: e.reciprocal(out=rd, in_=pD), [("ps", bD)], [("rden", ib)])
                self.dve(lambda e, qv=qv, pO=pO, rd=rd: e.tensor_tensor(out=qv, in0=pO, in1=rd, op=ALU.mult),
                         [("ps", bO), ("rden", ib)], [qtok])

    def wo(self, tile):
        S = self.S
        subs = self.subs(tile, False)
        S.barrier()
        self.carve_reset(self.akeep)
        self.sq = {0: self.carve(BF16, [KC, SUBW]), 1: self.carve(BF16, [KC, SUBW])}
        self.ysb = {0: self.carve(F32, [KC, SUBW]), 1: self.carve(F32, [KC, SUBW])}
        self.carve_common(subs)
        C = KC
        wsrc = self.d_wo[0]
        slots = {0: self.load_wd(wsrc, C, 0)}
        for m in range(KC):
            if m + 1 < KC:
                slots[m + 1] = self.load_wd(wsrc, C, m + 1)
            slot = slots[m]
            for sub in subs:
                b = self.nextbank4()
                for c in range(C):
                    self.mm(self.bank(b)[:, :], self.wd[slot][:, c, :], self.QA[:, c, sub.xcol:sub.xcol + SUBW],
                            c == 0, c == C - 1,
                            [("wd", slot), ("qa", c, sub.xcol // 256), ("qa", c, sub.xcol // 256 + 1)], [("ps", b)])
                self.act(self.ysb[sub.name][:, m, :], self.bank(b)[:, :], AF.Identity, [("ps", b), "cst"],
                         [("ysb", sub.name, m)], scale=self.ccol(("mpost", 1), m))
                self.act(self.sq[sub.name][:, m, :], self.bank(b)[:, :], AF.Square, [("ps", b)],
                         [(("sq", sub.name), m)])
        self._post(subs)

    def build(self, A, B):
        S = self.S
        self.akeep = 0
        self.dma("sp", self.cst[:, 0:N_IN_COLS], self.d_consts, "cst", [], ["cst"])
        self.dma("sp", self.cst[:, CC.m["flag"]:CC.m["flag"] + 1], self.d_flag, "cst", [], ["cst"])
        self.dma("sp", self.ones[:, :], self.d_ones, "ones", [], ["ones"])
        self.dma("sp", self.ident[:, :], self.d_ident, "ident", [], ["ident"])
        self.epsc = self.nc.alloc_sbuf_tensor("sb_epsc", [128, 2], F32)
        e0 = self.epsc[:, 0:1]
        e1 = self.epsc[:, 1:2]
        self.dve(lambda e: e.memset(e0, RMS_EPS), [], ["epsc0"])
        self.dve(lambda e: e.memset(e1, LN_EPS), [], ["epsc1"])
        for l in range(2):
            for i in range(2):
                o = self.cst[:, CC.m[("fposth", l, i)]:CC.m[("fposth", l, i)] + KC]
                s_ = self.cst[:, CC.m[("fpost", l, i)]:CC.m[("fpost", l, i)] + KC]
                self.dve(lambda e, o=o, s_=s_: e.tensor_scalar(out=o, in0=s_, scalar1=0.5, scalar2=None,
                                                              op0=ALU.mult), ["cst"], ["cst"])
        o = self.cst[:, CC.m["boutg"]:CC.m["boutg"] + KC]
        a_ = self.cst[:, CC.m["bout"]:CC.m["bout"] + KC]
        b_ = self.cst[:, CC.m[("mpost", 0)]:CC.m[("mpost", 0)] + KC]
        self.dve(lambda e: e.tensor_tensor(out=o, in0=a_, in1=b_, op=ALU.mult), ["cst"], ["cst"])
        for blk in range(T // SUBW):
            self.dma("sp", self.X[:, :, blk * SUBW:(blk + 1) * SUBW], self.d_x[:, :, blk * SUBW:(blk + 1) * SUBW],
                     "xld%d" % blk, [], [("x", k, blk) for k in range(KC)])
        if A:
            self.dma("sp", self.xh[:, :, :], self.d_xh, "xh", [], [("xh", k) for k in range(KC)])
            self.ffn(0, 0, 0, True)
            self.ffn(0, 0, 1, False)
            self.conv(0)
            self.conv(1)
            self.ffn(0, 1, 0, False)
            self.ffn(0, 1, 1, False)
            self.ffn(1, 0, 0, False)
            self.ffn(1, 0, 1, False)
        if self.mode == "A":
            self.kvq(0, True, False)
            self.kvq(1, True, False)
        if self.mode == "B":
            for nm, dst, src in (("cE", self.cE, self.d_E), ("ccausal", self.ccausal, self.d_causal),
                                 ("cabias", self.cabias, self.d_abias), ("crowv", self.crowv, self.d_rowv),
                                 ("cpmask", self.cpmask, self.d_pst), ("cgmask", self.cgmask, self.d_gmask)):
                self.dma("sp", dst[:], src, nm, [], [nm])
            pm = self.cpmask[:, :, :]
            gmb = self.cgmask[:, :].unsqueeze(1).to_broadcast([128, 8, 16])
            self.dve(lambda e: e.tensor_tensor(out=pm, in0=pm, in1=gmb, op=ALU.add), ["cpmask", "cgmask"],
                     ["cpmask"])
            self.QA = self.carve(BF16, [NH, T])
            self.akeep = self.aoff
            self.kvq(0, False, True)
            self.kvq(1, False, True)
            self.attention()
            self.wo(0)
            self.wo(1)
            self.akeep = 0
            self.ffn(1, 1, 0, False)
            self.ffn(1, 1, 1, False)
        for blk in range(T // SUBW):
            self.dma("sp", self.d_xout[:, :, blk * SUBW:(blk + 1) * SUBW], self.X[:, :, blk * SUBW:(blk + 1) * SUBW],
                     "xst%d" % blk, [("x", k, blk) for k in range(KC)], [])
        S.barrier(engines=("sp",))
        S.add("sp", None)


_PROG_CACHE = {}


def get_prog(mode):
    if mode not in _PROG_CACHE:
        _PROG_CACHE[mode] = Prog(mode)
    return _PROG_CACHE[mode]


def to_fm(xs):
    t = xs.shape[0]
    return np.ascontiguousarray(xs.reshape(t, KC, 128).transpose(2, 1, 0))


def from_fm(a):
    t = a.shape[2]
    return np.ascontiguousarray(a.transpose(2, 1, 0).reshape(t, D))


def kernel(**inp):
    inp = {k: np.asarray(v) for k, v in inp.items()}
    x = inp["x"].astype(np.float32)
    ncore = 8
    consts = host_consts(inp)
    ac = attn_consts()
    bf = ml_dtypes.bfloat16
    wts = {k: np.ascontiguousarray(inp[k], dtype=np.float32) for k in
           ("ffn_w_gate_up", "ffn_w_down", "conv_w_in", "conv_w_out", "w_kv", "attn_w_q", "attn_w_o")}
    pa = get_prog("A")
    in_maps = []
    for c in range(ncore):
        b, half = c // 2, c % 2
        xs = x[b, half * T:(half + 1) * T]
        m = dict(xin=to_fm(xs), consts=consts, c_ones=ac["c_ones"], c_ident=ac["c_ident"],
                 ffn_w_gate_up=wts["ffn_w_gate_up"], ffn_w_down=wts["ffn_w_down"],
                 conv_w_in=wts["conv_w_in"], conv_w_out=wts["conv_w_out"], w_kv=wts["w_kv"])
        if half == 1:
            m["xh"] = to_fm(x[b, T - HALO:T])
            m["flag"] = np.ones((128, 1), np.float32)
        else:
            m["xh"] = np.zeros((128, KC, HALO), np.float32)
            m["flag"] = np.zeros((128, 1), np.float32)
        in_maps.append(m)
    ra = run_bass_kernel_spmd(pa.nc, in_maps, core_ids=list(range(ncore))).results
    pb = get_prog("B")
    in_maps = []
    for c in range(ncore):
        b, half = c // 2, c % 2
        kown = np.asarray(ra[c]["kout"])
        vown = np.asarray(ra[c]["vout"])
        if half == 1:
            kpart = np.asarray(ra[c - 1]["kout"])
            vpart = np.asarray(ra[c - 1]["vout"])
            gmask = np.zeros((128, 16), np.float32)
        else:
            kpart = np.zeros_like(kown)
            vpart = np.zeros_like(vown)
            gmask = np.zeros((128, 16), np.float32)
            gmask[:, :8] = BIG
        m = dict(xin=np.asarray(ra[c]["xa"]), consts=consts, flag=np.zeros((128, 1), np.float32),
                 c_ones=ac["c_ones"], c_ident=ac["c_ident"], c_E=ac["c_E"], c_causal=ac["c_causal"],
                 c_abias=ac["c_abias"], c_rowv=ac["c_rowv"], c_pst=ac["c_pst"], gmask=gmask,
                 ffn_w_gate_up=wts["ffn_w_gate_up"], ffn_w_down=wts["ffn_w_down"],
                 attn_w_q=wts["attn_w_q"], attn_w_o=wts["attn_w_o"],
                 kin=np.ascontiguousarray(np.concatenate([kpart, kown], axis=2)),
                 vin=np.ascontiguousarray(np.concatenate([vpart, vown], axis=0)))
        in_maps.append(m)
    rb = run_bass_kernel_spmd(pb.nc, in_maps, core_ids=list(range(ncore))).results
    out = np.zeros((4, 2 * T, D), np.float32)
    for c in range(ncore):
        b, half = c // 2, c % 2
        out[b, half * T:(half + 1) * T] = from_fm(np.asarray(rb[c]["xout"]))
    return out
```
